# Optimizing a Trainium2 kernel written in Bass

```python
import numpy as np
import jax
import jax.numpy as jnp
from jax import lax

D_MODEL = 2048
BATCH = 1
SEQ = 8192
DEPTH = 2
DEC_BATCH = 2
DEC_SEQ = 16384
PAST_LEN = 128

N_EVEN = (DEPTH + 1) // 2
N_ODD = DEPTH // 2

MIX_A = D_MODEL // 2
RWKV_HEAD = 64
RWKV_HEADS = MIX_A // RWKV_HEAD
DECAY_LORA = 64
ICL_LORA = 64
GATE_LORA = 128
RWKV_SPLITS = (MIX_A, MIX_A, MIX_A, DECAY_LORA, DECAY_LORA, ICL_LORA, ICL_LORA, GATE_LORA)
RWKV_COLS = sum(RWKV_SPLITS)

MIX_B = D_MODEL - MIX_A
ATT_HEAD = 128
ATT_Q_HEADS = MIX_B // ATT_HEAD
ATT_KV_HEADS = 2
ATT_GROUP = ATT_Q_HEADS // ATT_KV_HEADS
ATT_KV_WIDTH = ATT_KV_HEADS * ATT_HEAD
WINDOW = 128
ATT_BLOCK = 128
ROPE_THETA = 10000.0
PROJ_EVEN = RWKV_COLS + MIX_B + 2 * ATT_KV_WIDTH

MIX_C = D_MODEL // 2
CONV_C = 3
MIX_D = D_MODEL - MIX_C
CONV_D = 4
LRU_BLOCKS = 16
LRU_BLOCK = MIX_D // LRU_BLOCKS
LRU_C = 8.0
PROJ_ODD = 3 * MIX_C + 2 * MIX_D

FFN_DENSE = 5632
N_EXPERTS = 8
TOP_K = 2
FFN_EXPERT = 7168
MOE_BLOCK = 256

DN_ALPHA = (2 * DEPTH) ** 0.25
DN_BETA = (8 * DEPTH) ** -0.25
LN_EPS = 1e-5
GN_EPS = 64e-5
NEG_INF = -1e30

kernel_name = 'hybrid_bidir_rwkv7_swa_conv_rglru_encoder'


def _split(x, sizes):
    idx = np.cumsum(sizes)[:-1].tolist()
    return jnp.split(x, idx, axis=-1)


def layer_norm(x, g, b):
    xf = x.astype(jnp.float32)
    mu = xf.mean(-1, keepdims=True)
    var = jnp.mean(jnp.square(xf - mu), -1, keepdims=True)
    return ((xf - mu) * lax.rsqrt(var + LN_EPS) * g + b).astype(x.dtype)


def shift_prev(x):
    return jnp.pad(x, ((0, 0), (1, 0), (0, 0)))[:, :-1]


def shift_next(x):
    return jnp.pad(x, ((0, 0), (0, 1), (0, 0)))[:, 1:]


def rotary(x):
    T = x.shape[1]
    half = ATT_HEAD // 2
    inv_freq = ROPE_THETA ** (-jnp.arange(half, dtype=jnp.float32) / half)
    ang = jnp.arange(T, dtype=jnp.float32)[:, None] * inv_freq[None, :]
    cos = jnp.cos(ang)[None, :, None, :]
    sin = jnp.sin(ang)[None, :, None, :]
    xf = x.astype(jnp.float32)
    x1, x2 = xf[..., :half], xf[..., half:]
    return jnp.concatenate([x1 * cos - x2 * sin, x2 * cos + x1 * sin], -1).astype(x.dtype)


def swiglu(x, w_gu, w_down):
    g, u = jnp.split(x @ w_gu, 2, axis=-1)
    return (jax.nn.silu(g) * u) @ w_down


def depthwise_conv(u, w, pad):
    return lax.conv_general_dilated(u, w[:, None, :].astype(u.dtype), (1,), [pad],
                                    dimension_numbers=('NWC', 'WIO', 'NWC'),
                                    feature_group_count=u.shape[-1])


def rwkv_scan(r, w, k, v, a, b, reverse):
    bsz = r.shape[0]

    def step(S, inp):
        r_t, w_t, k_t, v_t, a_t, b_t = inp
        sa = jnp.einsum('bhij,bhj->bhi', S, a_t)
        S = S * w_t[:, :, None, :] + sa[..., None] * b_t[:, :, None, :] + v_t[..., None] * k_t[:, :, None, :]
        return S, jnp.einsum('bhij,bhj->bhi', S, r_t)

    xs = tuple(jnp.moveaxis(z, 1, 0) for z in (r, w, k, v, a, b))
    S0 = jnp.zeros((bsz, RWKV_HEADS, RWKV_HEAD, RWKV_HEAD), jnp.float32)
    _, y = lax.scan(step, S0, xs, reverse=reverse)
    return jnp.moveaxis(y, 0, 1)


def rwkv7_bidir(p, w0, w2, a0, a2, g2, k_k, k_a, r_k, gn_g, gn_b):
    bsz, T, _ = p.shape
    r, k, v, wd_f, wd_b, ad_f, ad_b, gd = _split(p, RWKV_SPLITS)

    def heads(z):
        return z.astype(jnp.float32).reshape(bsz, T, RWKV_HEADS, RWKV_HEAD)

    rh, vh = heads(r), heads(v)
    kk = heads(k * k_k)
    kk = kk * lax.rsqrt(jnp.maximum(jnp.sum(kk * kk, -1, keepdims=True), 1e-24))
    ys, bonuses = [], []
    for d, (wd, ad) in enumerate(((wd_f, ad_f), (wd_b, ad_b))):
        w_pre = (w0[d] + jnp.tanh(wd) @ w2[d]).astype(jnp.float32)
        decay = jnp.exp(-jnp.exp(-jax.nn.softplus(-w_pre) - 0.5))
        icl = jax.nn.sigmoid((a0[d] + ad @ a2[d]).astype(jnp.float32))
        kd = heads(k * (1.0 + (icl - 1.0) * k_a))
        ah = heads(icl)
        ys.append(rwkv_scan(rh, heads(decay), kd, vh, -kk, kk * ah, d == 1))
        bonuses.append(jnp.sum(rh * kd * r_k, -1, keepdims=True) * vh)
    y = ys[0] + ys[1]
    mu = y.mean(-1, keepdims=True)
    var = jnp.mean(jnp.square(y - mu), -1, keepdims=True)
    y = ((y - mu) * lax.rsqrt(var + GN_EPS)).reshape(bsz, T, MIX_A) * gn_g + gn_b
    y = y + (bonuses[0] + bonuses[1]).reshape(bsz, T, MIX_A)
    g = jax.nn.sigmoid(gd) @ g2
    return (y * g).astype(p.dtype)


def window_gqa(q, k, v, sink):
    bsz, T = q.shape[:2]
    nb = T // ATT_BLOCK
    q = rotary(q)
    k = rotary(k)
    qb = q.reshape(bsz, nb, ATT_BLOCK, ATT_KV_HEADS, ATT_GROUP, ATT_HEAD)

    def band(z):
        zp = jnp.pad(z, ((0, 0), (ATT_BLOCK, ATT_BLOCK), (0, 0), (0, 0)))
        zp = zp.reshape(bsz, nb + 2, ATT_BLOCK, ATT_KV_HEADS, ATT_HEAD)
        return jnp.concatenate([zp[:, :-2], zp[:, 1:-1], zp[:, 2:]], axis=2)

    kw, vw = band(k), band(v)
    s = jnp.einsum('bnqhgd,bnchd->bnhgqc', qb, kw,
                   preferred_element_type=jnp.float32) * (ATT_HEAD ** -0.5)
    qi = jnp.arange(ATT_BLOCK)[:, None]
    ci = jnp.arange(3 * ATT_BLOCK)[None, :]
    kpos = (jnp.arange(nb)[:, None, None] - 1) * ATT_BLOCK + ci[None]
    mask = (jnp.abs(ci - ATT_BLOCK - qi) <= WINDOW)[None] & (kpos >= 0) & (kpos < T)
    s = jnp.where(mask[None, :, None, None], s, NEG_INF)
    sk = sink.astype(jnp.float32).reshape(1, 1, ATT_KV_HEADS, ATT_GROUP, 1)
    m = jnp.maximum(s.max(-1), sk)
    p = jnp.exp(s - m[..., None])
    denom = p.sum(-1) + jnp.exp(sk - m)
    o = jnp.einsum('bnhgqc,bnchd->bnqhgd', p, vw.astype(jnp.float32))
    o = o / jnp.transpose(denom, (0, 1, 4, 2, 3))[..., None]
    return o.reshape(bsz, T, MIX_B).astype(q.dtype)


def _lin_combine(c1, c2):
    a1, b1 = c1
    a2, b2 = c2
    return a1 * a2, a2 * b1 + b2


def rglru(xc, wr, br, wi, bi, lam, reverse):
    bsz, T, _ = xc.shape
    xf = xc.astype(jnp.float32)
    xb = xf.reshape(bsz, T, LRU_BLOCKS, LRU_BLOCK)
    r = jax.nn.sigmoid(jnp.einsum('btni,nij->btnj', xb, wr).reshape(bsz, T, MIX_D) + br)
    i = jax.nn.sigmoid(jnp.einsum('btni,nij->btnj', xb, wi).reshape(bsz, T, MIX_D) + bi)
    log_a = -LRU_C * r * jax.nn.softplus(-lam.astype(jnp.float32))
    a = jnp.exp(log_a)
    b = jnp.sqrt(-jnp.expm1(2.0 * log_a)) * (i * xf)
    if reverse:
        a, b = jnp.flip(a, 1), jnp.flip(b, 1)
    _, h = lax.associative_scan(_lin_combine, (a, b), axis=1)
    if reverse:
        h = jnp.flip(h, 1)
    return h


def moe_swiglu(x, router, w_gu, w_down):
    n = x.shape[0]
    logits = jnp.dot(x, router, preferred_element_type=jnp.float32)
    top_logit, top_idx = lax.top_k(logits, TOP_K)
    gates = jax.nn.softmax(top_logit, axis=-1).astype(x.dtype)
    flat_e = top_idx.reshape(-1)
    flat_tok = jnp.repeat(jnp.arange(n, dtype=jnp.int32), TOP_K)
    flat_g = gates.reshape(-1)
    order = jnp.argsort(flat_e)
    se, stok, sg = flat_e[order], flat_tok[order], flat_g[order]
    counts = jnp.bincount(flat_e, length=N_EXPERTS)
    padded = (counts + MOE_BLOCK - 1) // MOE_BLOCK * MOE_BLOCK
    start = jnp.cumsum(counts) - counts
    pend = jnp.cumsum(padded)
    pstart = pend - padded
    dest = pstart[se] + (jnp.arange(n * TOP_K, dtype=jnp.int32) - start[se])
    n_blocks = -(-(n * TOP_K) // MOE_BLOCK) + N_EXPERTS
    slots = n_blocks * MOE_BLOCK
    slot_tok = jnp.full((slots,), n, jnp.int32).at[dest].set(stok)
    slot_gate = jnp.zeros((slots,), x.dtype).at[dest].set(sg)
    block_e = jnp.minimum(jnp.searchsorted(pend, jnp.arange(n_blocks) * MOE_BLOCK, side='right'),
                          N_EXPERTS - 1)
    x_pad = jnp.concatenate([x, jnp.zeros((1, x.shape[1]), x.dtype)], 0)
    xs = x_pad[slot_tok].reshape(n_blocks, MOE_BLOCK, x.shape[1])

    def expert_block(args):
        xb, e = args
        return swiglu(xb, w_gu[e], w_down[e])

    ys = lax.map(expert_block, (xs, block_e)).reshape(slots, x.shape[1])
    out = jax.ops.segment_sum(ys * slot_gate[:, None], slot_tok, num_segments=n + 1)
    return out[:n]


def even_layer(x, ln_g, ln_b, w_in, w_out, mu, w0, w2, a0, a2, g2, k_k, k_a, r_k,
               gn_g, gn_b, sink, ffn_gu, ffn_down):
    bsz, T, _ = x.shape
    proj = x @ w_in
    p_rw, q, k, v = _split(proj, (RWKV_COLS, MIX_B, ATT_KV_WIDTH, ATT_KV_WIDTH))
    p_rw = p_rw + mu[0] * (shift_prev(p_rw) - p_rw) + mu[1] * (shift_next(p_rw) - p_rw)
    y_a = rwkv7_bidir(p_rw, w0, w2, a0, a2, g2, k_k, k_a, r_k, gn_g, gn_b)
    y_b = window_gqa(q.reshape(bsz, T, ATT_Q_HEADS, ATT_HEAD),
                     k.reshape(bsz, T, ATT_KV_HEADS, ATT_HEAD),
                     v.reshape(bsz, T, ATT_KV_HEADS, ATT_HEAD), sink)
    mix = jnp.concatenate([y_a, y_b], -1) @ w_out
    x = layer_norm(DN_ALPHA * x + mix, ln_g[0], ln_b[0])
    x = layer_norm(DN_ALPHA * x + swiglu(x, ffn_gu, ffn_down), ln_g[1], ln_b[1])
    return x


def odd_layer(x, ln_g, ln_b, w_in, w_out, sc_w, conv_w, conv_b, wr, br, wi, bi, lam,
              router, e_gu, e_down):
    bsz, T, _ = x.shape
    proj = x @ w_in
    h, gb, gc, xr, gate = _split(proj, (MIX_C, MIX_C, MIX_C, MIX_D, MIX_D))
    y_c = gb * depthwise_conv(gc * h, sc_w, (CONV_C // 2, CONV_C // 2))
    xc = depthwise_conv(xr, conv_w, (CONV_D // 2, CONV_D - 1 - CONV_D // 2)) + conv_b
    hsum = (rglru(xc, wr[0], br[0], wi[0], bi[0], lam[0], False)
            + rglru(xc, wr[1], br[1], wi[1], bi[1], lam[1], True))
    y_d = (hsum * jax.nn.gelu(gate.astype(jnp.float32))).astype(x.dtype)
    mix = jnp.concatenate([y_c, y_d], -1) @ w_out
    x = layer_norm(DN_ALPHA * x + mix, ln_g[0], ln_b[0])
    ff = moe_swiglu(x.reshape(bsz * T, D_MODEL), router, e_gu, e_down).reshape(bsz, T, D_MODEL)
    x = layer_norm(DN_ALPHA * x + ff, ln_g[1], ln_b[1])
    return x


def setup_inputs(seed: int = 0) -> dict:
    key = jax.random.key(seed)
    ks = iter(jax.random.split(key, 48))
    f32 = jnp.float32

    def nrm(shape, scale):
        return jax.random.normal(next(ks), shape, f32) * scale

    def unif(shape, lo, hi):
        return jax.random.uniform(next(ks), shape, f32, lo, hi)

    lru_a = unif((N_ODD, 2, MIX_D), 0.9, 0.999) ** (1.0 / LRU_C)
    return {
        'x_prompt': nrm((BATCH, SEQ, D_MODEL), 1.0),
        'x_sample': nrm((DEC_BATCH, DEC_SEQ, D_MODEL), 1.0),
        'ln_g': 1.0 + nrm((DEPTH, 2, D_MODEL), 0.02),
        'ln_b': nrm((DEPTH, 2, D_MODEL), 0.02),
        'ev_w_in': nrm((N_EVEN, D_MODEL, PROJ_EVEN), D_MODEL ** -0.5),
        'ev_w_out': nrm((N_EVEN, D_MODEL, D_MODEL), DN_BETA * D_MODEL ** -0.5),
        'rw_mu': unif((N_EVEN, 2, RWKV_COLS), 0.0, 0.4),
        'rw_w0': unif((N_EVEN, 2, MIX_A), -6.0, -1.0),
        'rw_w2': nrm((N_EVEN, 2, DECAY_LORA, MIX_A), 0.1 * DECAY_LORA ** -0.5),
        'rw_a0': nrm((N_EVEN, 2, MIX_A), 0.1),
        'rw_a2': nrm((N_EVEN, 2, ICL_LORA, MIX_A), 0.3 * ICL_LORA ** -0.5),
        'rw_g2': nrm((N_EVEN, GATE_LORA, MIX_A), GATE_LORA ** -0.5),
        'rw_kk': 0.85 + nrm((N_EVEN, MIX_A), 0.05),
        'rw_ka': 1.0 + nrm((N_EVEN, MIX_A), 0.05),
        'rw_rk': nrm((N_EVEN, RWKV_HEADS, RWKV_HEAD), 0.1),
        'rw_gn_g': 1.0 + nrm((N_EVEN, MIX_A), 0.02),
        'rw_gn_b': nrm((N_EVEN, MIX_A), 0.02),
        'att_sink': nrm((N_EVEN, ATT_Q_HEADS), 0.5),
        'ffn_w_gu': nrm((N_EVEN, D_MODEL, 2 * FFN_DENSE), D_MODEL ** -0.5),
        'ffn_w_down': nrm((N_EVEN, FFN_DENSE, D_MODEL), DN_BETA * FFN_DENSE ** -0.5),
        'od_w_in': nrm((N_ODD, D_MODEL, PROJ_ODD), D_MODEL ** -0.5),
        'od_w_out': nrm((N_ODD, D_MODEL, D_MODEL), DN_BETA * D_MODEL ** -0.5),
        'sc_conv': nrm((N_ODD, CONV_C, MIX_C), CONV_C ** -0.5),
        'lru_conv': nrm((N_ODD, CONV_D, MIX_D), CONV_D ** -0.5),
        'lru_conv_b': nrm((N_ODD, MIX_D), 0.02),
        'lru_wr': nrm((N_ODD, 2, LRU_BLOCKS, LRU_BLOCK, LRU_BLOCK), LRU_BLOCK ** -0.5),
        'lru_br': nrm((N_ODD, 2, MIX_D), 0.1),
        'lru_wi': nrm((N_ODD, 2, LRU_BLOCKS, LRU_BLOCK, LRU_BLOCK), LRU_BLOCK ** -0.5),
        'lru_bi': nrm((N_ODD, 2, MIX_D), 0.1),
        'lru_lam': jnp.log(lru_a) - jnp.log1p(-lru_a),
        'moe_router': nrm((N_ODD, D_MODEL, N_EXPERTS), D_MODEL ** -0.5),
        'moe_w_gu': nrm((N_ODD, N_EXPERTS, D_MODEL, 2 * FFN_EXPERT), D_MODEL ** -0.5),
        'moe_w_down': nrm((N_ODD, N_EXPERTS, FFN_EXPERT, D_MODEL), DN_BETA * FFN_EXPERT ** -0.5),
    }


def reference(x_prompt, x_sample, ln_g, ln_b, ev_w_in, ev_w_out, rw_mu, rw_w0, rw_w2, rw_a0,
              rw_a2, rw_g2, rw_kk, rw_ka, rw_rk, rw_gn_g, rw_gn_b, att_sink, ffn_w_gu,
              ffn_w_down, od_w_in, od_w_out, sc_conv, lru_conv, lru_conv_b, lru_wr, lru_br,
              lru_wi, lru_bi, lru_lam, moe_router, moe_w_gu, moe_w_down):
    def encoder(x):
        for layer in range(DEPTH):
            i = layer // 2
            if layer % 2 == 0:
                x = even_layer(x, ln_g[layer], ln_b[layer], ev_w_in[i], ev_w_out[i], rw_mu[i],
                               rw_w0[i], rw_w2[i], rw_a0[i], rw_a2[i], rw_g2[i], rw_kk[i],
                               rw_ka[i], rw_rk[i], rw_gn_g[i], rw_gn_b[i], att_sink[i],
                               ffn_w_gu[i], ffn_w_down[i])
            else:
                x = odd_layer(x, ln_g[layer], ln_b[layer], od_w_in[i], od_w_out[i], sc_conv[i],
                              lru_conv[i], lru_conv_b[i], lru_wr[i], lru_br[i], lru_wi[i],
                              lru_bi[i], lru_lam[i], moe_router[i], moe_w_gu[i], moe_w_down[i])
        return x

    y_prompt = encoder(x_prompt)
    y_sample = encoder(x_sample)
    return (y_prompt, y_sample)
```

```python
import numpy as np
import ml_dtypes
from contextlib import ExitStack
import concourse.bass as bass
import concourse.mybir as mybir
from concourse.bass_utils import run_bass_kernel_spmd

F32 = mybir.dt.float32
BF16 = mybir.dt.bfloat16
AF = mybir.ActivationFunctionType
ALU = mybir.AluOpType
AX = mybir.AxisListType

NCORES = 8
STOP = 0
EXP = 0
D = 2048
KC = D // 128
ENGS = ("pe", "act", "dve", "pool", "sp")


class Buf:
    __slots__ = ("t", "lw", "rd", "name")

    def __init__(self, t, name=""):
        self.t = t
        self.lw = None
        self.rd = {}
        self.name = name

    def __getitem__(self, idx):
        return self.t[idx]


class FW:
    def __init__(self, nc, es, n_dma_sems=32):
        self.nc = nc
        self.es = es
        self.q = {e: [] for e in ENGS}
        self.cnt = {e: 0 for e in ENGS}
        self.esem = {e: es.enter_context(nc.semaphore("s_" + e)) for e in ENGS}
        self.seen = {e: {} for e in ENGS}
        self.dsems = [es.enter_context(nc.semaphore("d%d" % i)) for i in range(n_dma_sems)]
        self.dcnt = [0] * n_dma_sems
        self.dlast = [None] * n_dma_sems
        self.dnext = 0
        self.force = {}

    def clear_all(self):
        sems = list(self.esem.values()) + list(self.dsems)
        with self.nc.Block() as block:
            @block.sync
            def _(e):
                for s in sems:
                    e.sem_clear(s)

    def begin(self):
        self._outer = self.es
        self.es = ExitStack()

    def end(self):
        self.emit()
        self.es.close()
        self.es = self._outer

    def sbuf(self, name, shape, dt):
        self.uid = getattr(self, "uid", 0) + 1
        return Buf(self.es.enter_context(self.nc.sbuf_tensor("sb%d_%s" % (self.uid, name), shape, dt)), name)

    def psum(self, name, shape, dt=F32):
        return Buf(self.es.enter_context(self.nc.psum_tensor(name, shape, dt)), name)

    def dram(self, name, shape, dt, kind="Internal"):
        return Buf(self.nc.dram_tensor(name, shape, dt, kind=kind), name)

    def _sem_of(self, ev):
        kind, key, val = ev
        return (self.esem[key] if kind == "e" else self.dsems[key]), val

    def _need(self, eng, ev, waits):
        if ev is None:
            return
        kind, key, val = ev
        k = (kind, key)
        if self.seen[eng].get(k, -1) >= val:
            return
        self.seen[eng][k] = val
        waits.append(self._sem_of(ev))

    def _deps(self, eng, reads, writes):
        waits = []
        for ev in self.force.pop(eng, []):
            self._need(eng, ev, waits)
        for b in reads:
            self._need(eng, b.lw, waits)
        for b in writes:
            self._need(eng, b.lw, waits)
            for k, v in b.rd.items():
                self._need(eng, (k[0], k[1], v), waits)
        return waits

    def _commit(self, ev, reads, writes):
        for b in reads:
            b.rd[(ev[0], ev[1])] = ev[2]
        for b in writes:
            b.lw = ev
            b.rd = {}

    def op(self, eng, fn, reads=(), writes=()):
        waits = self._deps(eng, reads, writes)
        self.cnt[eng] += 1
        ev = ("e", eng, self.cnt[eng])
        self.q[eng].append((fn, waits, (self.esem[eng], 1)))
        self._commit(ev, reads, writes)
        return ev

    def dma(self, fn, reads=(), writes=(), eng="sp", inc=16):
        lo, n = (0, 6) if eng == "sp" else ((16, 12) if inc == 16 else (28, 4))
        pk = (eng, inc)
        if not hasattr(self, "dnx"):
            self.dnx = {}
        j = self.dnx.get(pk, 0)
        self.dnx[pk] = (j + 1) % n
        i = lo + j
        waits = self._deps(eng, reads, writes)
        self._need(eng, self.dlast[i], waits)
        self.dcnt[i] += inc
        ev = ("d", i, self.dcnt[i])
        self.dlast[i] = ev
        self.q[eng].append((fn, waits, (self.dsems[i], inc)))
        self._commit(ev, reads, writes)
        if inc == 1:
            self.force.setdefault(eng, []).append(ev)
        return ev

    def emit(self):
        nc = self.nc
        if not any(self.q[e] for e in ENGS):
            return
        fin = []
        for i, ev in enumerate(self.dlast):
            if ev is not None:
                fin.append(self._sem_of(ev))
        for e in ENGS:
            if e != "sp" and self.cnt[e] > 0:
                fin.append((self.esem[e], self.cnt[e]))
        q = self.q

        def run(engobj, lst, extra=()):
            for fn, waits, inc in lst:
                for s, v in waits:
                    engobj.wait_ge(s, v)
                fn(engobj).then_inc(inc[0], inc[1])
            for s, v in extra:
                engobj.wait_ge(s, v)

        with nc.Block() as block:
            @block.tensor
            def _(e):
                run(e, q["pe"])

            @block.scalar
            def _(e):
                run(e, q["act"])

            @block.vector
            def _(e):
                run(e, q["dve"])

            @block.gpsimd
            def _(e):
                run(e, q["pool"])

            @block.sync
            def _(e):
                run(e, q["sp"], fin)
        self.q = {e: [] for e in ENGS}


def _consts(lmax):
    s = np.arange(128)[:, None]
    t = np.arange(128)[None, :]
    c = {}
    c["ident_f"] = np.eye(128, dtype=np.float32)
    cums = np.zeros((128, 4, 128), np.float32)
    cums[:, 0] = s <= t
    cums[:, 1] = s < t
    cums[:, 2] = s >= t
    cums[:, 3] = s > t
    c["cums"] = cums
    msk = np.zeros((128, 2, 256), np.float32)
    msk[:, 0, :128] = s < t
    msk[:, 0, 128:] = s <= t
    msk[:, 1, :128] = s > t
    msk[:, 1, 128:] = s >= t
    c["msk"] = msk
    mskT = np.zeros((128, 2, 128), np.float32)
    mskT[:, 0] = s > t
    mskT[:, 1] = s < t
    c["mskT"] = mskT
    bm = np.zeros((128, 128), np.float32)
    bm[:64, :64] = 1
    bm[64:, 64:] = 1
    c["blockmask"] = bm
    q = np.arange(128)[:, None]
    cc = np.arange(384)[None, :]
    c["attmask"] = np.where((cc >= q) & (cc <= q + 256), 0.0, -1e30).astype(np.float32)
    half = 64
    inv_freq = (10000.0 ** (-np.arange(half, dtype=np.float32) / half)).astype(np.float32)
    ang = inv_freq[:, None] * np.arange(lmax, dtype=np.float32)[None, :]
    cos = np.cos(ang).astype(np.float32)
    sin = np.sin(ang).astype(np.float32)
    c["ropec"] = np.concatenate([cos, cos], 0)
    c["ropes"] = np.concatenate([-sin, sin], 0)
    c["ones_f"] = np.ones((128, 128), np.float32)
    return c


_BLOB_ITEMS = (("ident_f", 128), ("cums", 512), ("msk", 512), ("mskT", 256), ("blockmask", 128), ("attmask", 384),
               ("ones_f", 128), ("lnp", 128), ("sel", 8), ("mu_fm", 12), ("w2", 128), ("a2", 128), ("g2", 128), ("prm1", 16),
               ("lwr", 256), ("lwi", 256), ("router", 128), ("e8", 1024), ("prm0", 2048))


def _blob_layout(lmax):
    lay, c = {}, 0
    for name, n in _BLOB_ITEMS + (("ropec", lmax), ("ropes", lmax)):
        lay[name] = (c, n)
        c += n
    return lay, c


def _fm_vec(v):
    return np.ascontiguousarray(np.asarray(v, np.float32).reshape(-1, 128).T)


def build_program(seqs, tpc, F0, FE, TT, phase):
    NT = sum(seqs)
    assert NT == NCORES * tpc and tpc % TT == 0 and TT % 128 == 0
    lmax = max(seqs)
    nc = bass.Bass("TRN2", target_bir_lowering=False)
    es = ExitStack()
    fw = FW(nc, es)
    fw.clear_all()

    def din(name, shape, dt=F32):
        return Buf(nc.dram_tensor(name, list(shape), dt, kind="ExternalInput"), name)

    def dout(name, shape, dt=F32):
        return Buf(nc.dram_tensor(name, list(shape), dt, kind="ExternalOutput"), name)

    lay, blobM = _blob_layout(lmax)
    blob = din("blob", [128, blobM])

    def bv(name, shape, rows=128):
        c0, n = lay[name]
        ap = blob.t.ap()[0:rows, c0:c0 + n]
        if len(shape) == 3:
            ap = ap.rearrange("p (a b) -> p a b", a=shape[1])
        return Buf(ap, name)
    cn = {k: bv(k, s) for k, s in (("ident_f", [128, 128]), ("cums", [128, 4, 128]), ("msk", [128, 2, 256]), ("mskT", [128, 2, 128]),
                                   ("blockmask", [128, 128]), ("attmask", [128, 384]), ("ones_f", [128, 128]))}
    ropec = bv("ropec", [128, lmax])
    ropes = bv("ropes", [128, lmax])
    prm0 = bv("prm0", [1, 2048], rows=1)
    w2_in = bv("w2", [128, 128])
    a2_in = bv("a2", [128, 128])
    g2_in = bv("g2", [128, 128])
    lnp = bv("lnp", [128, 8 * KC])
    prm1 = bv("prm1", [128, 16])
    lwr_in = bv("lwr", [128, 2, 128])
    lwi_in = bv("lwi", [128, 2, 128])
    rt_in = bv("router", [128, KC, 8])

    def castdma(dst, src, rows, r0d=0):
        step = 256
        for r0 in range(0, rows, step):
            r1 = min(rows, r0 + step)
            fw.dma(lambda e, r0=r0, r1=r1: e.dma_start(out=dst[r0d + r0:r0d + r1, :], in_=src[r0:r1, :]),
                   reads=[src], writes=[dst], eng="pool")

    def cload(name, src, shape, dt=F32, view=None):
        b = fw.sbuf(name, shape, dt)
        fw.dma(lambda e: e.dma_start(out=b[:], in_=(view if view is not None else src[:])), reads=[src], writes=[b])
        return b

    ident_f = cload("ident_f", cn["ident_f"], [128, 128])
    ident_b = fw.sbuf("ident_b", [128, 128], BF16)
    fw.op("dve", lambda e: e.tensor_copy(ident_b[:], ident_f[:]), reads=[ident_f], writes=[ident_b])
    cums = cload("cums", cn["cums"], [128, 4, 128])
    msk = cload("msk", cn["msk"], [128, 2, 256])
    mskT = cload("mskT", cn["mskT"], [128, 2, 128])
    blockmask = cload("blockmask", cn["blockmask"], [128, 128])
    attmask = cload("attmask", cn["attmask"], [128, 384])
    ones_f = cload("ones_f", cn["ones_f"], [128, 128])
    lnp_s = cload("lnp_s", lnp, [128, 8 * KC])

    PS = [fw.psum("ps%d" % i, [128, 512], F32) for i in range(6)]
    PB = [fw.psum("pb%d" % i, [128, 1024], BF16) for i in range(2)]

    def mm(ps_ap, lhsT, rhs, start, stop, reads, writes):
        fw.op("pe", lambda e: e.matmul(ps_ap, lhsT, rhs, start=start, stop=stop), reads=reads, writes=writes)

    def dve(fn, reads, writes):
        fw.op("dve", fn, reads=reads, writes=writes)

    def act(fn, reads, writes):
        fw.op("act", fn, reads=reads, writes=writes)

    def pool(fn, reads, writes):
        fw.op("pool", fn, reads=reads, writes=writes)

    def ld(out_ap, in_ap, reads, writes):
        fw.dma(lambda e: e.dma_start(out=out_ap, in_=in_ap), reads=reads, writes=writes)

    def ldc(out_ap, in_ap, reads, writes):
        fw.dma(lambda e: e.dma_start(out=out_ap, in_=in_ap), reads=reads, writes=writes, eng="pool")

    soff = [0]
    for L in seqs:
        soff.append(soff[-1] + L)
    if phase == "B0":
        xall = din("xall", [D, NT])
        w0_in = din("w0", [D, 1408])
        w0b = fw.dram("w0b", [D, 1408], BF16)
        castdma(w0b, w0_in, D)
        YTl = dout("yt", [256, NT])
        YF = fw.dram("YF", [NT, 256], F32)
        mu_fm_in = bv("mu_fm", [128, 12])
        mufm = fw.sbuf("mufm", [128, 6, 3], F32)
        mutmp = cload("mutmp", mu_fm_in, [128, 12])
        for g in range(6):
            dve(lambda e, g=g: e.tensor_copy(mufm[:, g, 1:3], mutmp[:, 2 * g:2 * g + 2]), [mutmp], [mufm])
            dve(lambda e, g=g: e.tensor_tensor(out=mufm[:, g, 0:1], in0=mutmp[:, 2 * g:2 * g + 1], in1=mutmp[:, 2 * g + 1:2 * g + 2], op=ALU.add), [mutmp], [mufm])
            dve(lambda e, g=g: e.tensor_scalar(out=mufm[:, g, 0:1], in0=mufm[:, g, 0:1], scalar1=-1.0, scalar2=1.0, op0=ALU.mult, op1=ALU.add), [mufm], [mufm])
        prm = fw.sbuf("prm", [128, 16, 128], F32)
        ld(prm[:].rearrange("p r n -> p (r n)"), prm0[:].to_broadcast([128, 16 * 128]), [prm0], [prm])
        dve(lambda e: e.tensor_scalar(out=prm[:, 9, :], in0=prm[:, 5, :], scalar1=-1.0, scalar2=1.0, op0=ALU.mult, op1=ALU.add), [prm], [prm])
        sinkb = prm[:, 10, 0:1]
        Wall = fw.sbuf("Wall", [128, KC, 1408], BF16)
        ld(Wall[:], w0b.t.ap().rearrange("(kc p) n -> p kc n", p=128), [w0b], [Wall])

        def load_bf(name, src):
            t32 = cload(name + "_32", src, [128, 128])
            tb = fw.sbuf(name, [128, 128], BF16)
            dve(lambda e: e.tensor_copy(tb[:], t32[:]), [t32], [tb])
            return tb
        w2b = load_bf("w2b", w2_in)
        a2b = load_bf("a2b", a2_in)
        g2b = load_bf("g2b", g2_in)

        XTv = xall.t.ap().rearrange("(kc p) t -> p kc t", p=128)
        xts = [fw.sbuf("xts%d" % i, [128, KC, 130], BF16) for i in range(2)]
        xctr = [0]

        def load_xt(g0, s0, s1):
            xt = xts[xctr[0] % 2]
            xctr[0] += 1
            lo, hi = max(s0, g0 - 1), min(s1, g0 + 129)
            if lo > g0 - 1:
                pool(lambda e, xt=xt: e.memset(xt[:, :, 0:1], 0.0), [], [xt])
            if hi < g0 + 129:
                pool(lambda e, xt=xt: e.memset(xt[:, :, 129:130], 0.0), [], [xt])
            ldc(xt[:, :, lo - (g0 - 1):hi - (g0 - 1)], XTv[:, :, lo:hi], [xall], [xt])
            return xt

        def T32(name, w=128):
            return fw.sbuf(name, [128, w], F32)

        def TB(name, w=128):
            return fw.sbuf(name, [128, w], BF16)
        fmg = [T32("fmg%d" % g) for g in range(6)]
        tanhwd, adb, siggd = TB("tanhwd"), TB("adb"), TB("siggd")
        r_t, k_t, v_t = T32("r_t"), T32("k_t"), T32("v_t")
        v_b = TB("v_b")
        lw, icl, t1, kd, kkn, bq, sqk = T32("lw"), T32("icl"), T32("t1"), T32("kd"), T32("kkn"), T32("bq"), T32("sqk")
        ss = fw.sbuf("ss", [128, 8], F32)
        Pm, Pinv, Pm1 = T32("Pm"), T32("Pinv"), T32("Pm1")
        rt_b, at_b, bt_b, kt_b = TB("rt_b"), TB("at_b"), TB("bt_b"), TB("kt_b")
        arbk = TB("arbk", 512)
        ptot = fw.sbuf("ptot", [128, 1], F32)
        Am = [TB("Am%d" % h, 512) for h in range(2)]
        Nn = [[TB("N%d_%d" % (h, i)) for i in range(2)] for h in range(2)]
        NTt = [[TB("NT%d_%d" % (h, i)) for i in range(2)] for h in range(2)]
        Rr = [[TB("R%d_%d" % (h, i)) for i in range(2)] for h in range(2)]
        XTs, UTs = TB("XTs"), TB("UTs")
        S32, STb = T32("S32"), TB("STb")
        yb = T32("yb", 256)
        yf = T32("yf", 256)
        gno = T32("gno")
        gnob = TB("gnob")
        ytb = T32("ytb")
        bnst = fw.sbuf("bnst", [128, 2, 6], F32)
        bnag = fw.sbuf("bnag", [128, 2, 2], F32)
        NEG_EH = -float(np.exp(-0.5))

        def rwkv_chunk(d, g0, s0, s1):
            hs = [slice(0, 64), slice(64, 128)]
            xt = load_xt(g0, s0, s1)
            for g in range(6):
                if g == 5 and d == 0:
                    continue
                ps = PS[g % 2]
                for kc in range(KC):
                    mm(ps[:, 0:130], Wall[:, kc, g * 128:(g + 1) * 128], xt[:, kc, :], kc == 0, kc == KC - 1, [Wall, xt], [ps])
                o = fmg[g]
                dve(lambda e, o=o, ps=ps, g=g: e.tensor_scalar(out=o[:], in0=ps[:, 1:129], scalar1=mufm[:, g, 0:1], scalar2=None, op0=ALU.mult), [ps, mufm], [o])
                dve(lambda e, o=o, ps=ps, g=g: e.scalar_tensor_tensor(out=o[:], in0=ps[:, 0:128], scalar=mufm[:, g, 1:2], in1=o[:], op0=ALU.mult, op1=ALU.add), [ps, mufm, o], [o])
                dve(lambda e, o=o, ps=ps, g=g: e.scalar_tensor_tensor(out=o[:], in0=ps[:, 2:130], scalar=mufm[:, g, 2:3], in1=o[:], op0=ALU.mult, op1=ALU.add), [ps, mufm, o], [o])
            act(lambda e: e.activation(out=tanhwd[:], in_=fmg[3][:], func=AF.Tanh), [fmg[3]], [tanhwd])
            act(lambda e: e.copy(adb[:], fmg[4][:]), [fmg[4]], [adb])
            for i, dst in enumerate((r_t, k_t, v_t)):
                ps = PS[2]
                fw.op("pe", lambda e, i=i, ps=ps: e.transpose(ps[:, 0:128], fmg[i][:], ident_f[:]), reads=[fmg[i], ident_f], writes=[ps])
                dve(lambda e, dst=dst, ps=ps: e.tensor_copy(dst[:], ps[:, 0:128]), [ps], [dst])
            act(lambda e: e.copy(v_b[:], v_t[:]), [v_t], [v_b])
            dsl = slice(64 * d, 64 * d + 64)
            ps = PS[3]
            mm(ps[:, 0:128], tanhwd[dsl, :], w2b[dsl, :], True, True, [tanhwd, w2b], [ps])
            dve(lambda e: e.tensor_tensor(out=lw[:], in0=ps[:, 0:128], in1=prm[:, 0 + d, :], op=ALU.add), [ps, prm], [lw])
            act(lambda e: e.activation(out=lw[:], in_=lw[:], func=AF.Sigmoid), [lw], [lw])
            dve(lambda e: e.tensor_scalar(out=lw[:], in0=lw[:], scalar1=NEG_EH, scalar2=None, op0=ALU.mult), [lw], [lw])
            mm(ps[:, 128:256], adb[dsl, :], a2b[dsl, :], True, True, [adb, a2b], [ps])
            dve(lambda e: e.tensor_tensor(out=icl[:], in0=ps[:, 128:256], in1=prm[:, 2 + d, :], op=ALU.add), [ps, prm], [icl])
            act(lambda e: e.activation(out=icl[:], in_=icl[:], func=AF.Sigmoid), [icl], [icl])
            dve(lambda e: e.tensor_tensor(out=t1[:], in0=icl[:], in1=prm[:, 5, :], op=ALU.mult), [icl, prm], [t1])
            dve(lambda e: e.tensor_tensor(out=t1[:], in0=t1[:], in1=prm[:, 9, :], op=ALU.add), [t1, prm], [t1])
            dve(lambda e: e.tensor_tensor(out=kd[:], in0=k_t[:], in1=t1[:], op=ALU.mult), [k_t, t1], [kd])
            dve(lambda e: e.tensor_tensor(out=kkn[:], in0=k_t[:], in1=prm[:, 4, :], op=ALU.mult), [k_t, prm], [kkn])
            dve(lambda e: e.tensor_tensor(out=sqk[:], in0=kkn[:], in1=kkn[:], op=ALU.mult), [kkn], [sqk])
            dve(lambda e: e.tensor_reduce(out=ss[:, 0:2], in_=sqk[:].rearrange("p (h j) -> p h j", h=2), axis=AX.X, op=ALU.add), [sqk], [ss])
            dve(lambda e: e.tensor_scalar(out=ss[:, 0:2], in0=ss[:, 0:2], scalar1=1e-24, scalar2=None, op0=ALU.max), [ss], [ss])
            act(lambda e: e.activation(out=ss[:, 2:4], in_=ss[:, 0:2], func=AF.Sqrt), [ss], [ss])
            dve(lambda e: e.reciprocal(ss[:, 0:2], ss[:, 2:4]), [ss], [ss])
            for h in range(2):
                dve(lambda e, h=h: e.tensor_scalar(out=kkn[:, hs[h]], in0=kkn[:, hs[h]], scalar1=ss[:, h:h + 1], scalar2=None, op0=ALU.mult), [kkn, ss], [kkn])
            dve(lambda e: e.tensor_tensor(out=bq[:], in0=kkn[:], in1=icl[:], op=ALU.mult), [kkn, icl], [bq])
            dve(lambda e: e.tensor_tensor(out=sqk[:], in0=r_t[:], in1=kd[:], op=ALU.mult), [r_t, kd], [sqk])
            dve(lambda e: e.tensor_tensor(out=sqk[:], in0=sqk[:], in1=prm[:, 6, :], op=ALU.mult), [sqk, prm], [sqk])
            dve(lambda e: e.tensor_reduce(out=ss[:, 4:6], in_=sqk[:].rearrange("p (h j) -> p h j", h=2), axis=AX.X, op=ALU.add), [sqk], [ss])
            for h in range(2):
                dve(lambda e, h=h: e.tensor_scalar(out=yb[:, 128 + 64 * h:192 + 64 * h], in0=v_t[:, hs[h]], scalar1=ss[:, 4 + h:5 + h], scalar2=None, op0=ALU.mult), [v_t, ss], [yb])
            ps = PS[3]
            mm(ps[:, 256:384], cums[:, 2 * d, :], lw[:], True, True, [cums, lw], [ps])
            mm(ps[:, 384:512], cums[:, 2 * d + 1, :], lw[:], True, True, [cums, lw], [ps])
            act(lambda e: e.activation(out=Pm[:], in_=ps[:, 256:384], func=AF.Exp), [ps], [Pm])
            act(lambda e: e.activation(out=Pinv[:], in_=ps[:, 256:384], func=AF.Exp, scale=-1.0), [ps], [Pinv])
            act(lambda e: e.activation(out=Pm1[:], in_=ps[:, 384:512], func=AF.Exp), [ps], [Pm1])
            ps2 = PS[2]
            mm(ps2[:, 128:129], lw[:], ones_f[:, 0:1], True, True, [lw, ones_f], [ps2])
            act(lambda e: e.activation(out=ptot[:], in_=ps2[:, 128:129], func=AF.Exp), [ps2], [ptot])
            dve(lambda e: e.tensor_tensor(out=rt_b[:], in0=r_t[:], in1=Pm[:], op=ALU.mult), [r_t, Pm], [rt_b])
            dve(lambda e: e.scalar_tensor_tensor(out=at_b[:], in0=kkn[:], scalar=-1.0, in1=Pm1[:], op0=ALU.mult, op1=ALU.mult), [kkn, Pm1], [at_b])
            dve(lambda e: e.tensor_tensor(out=bt_b[:], in0=bq[:], in1=Pinv[:], op=ALU.mult), [bq, Pinv], [bt_b])
            dve(lambda e: e.tensor_tensor(out=kt_b[:], in0=kd[:], in1=Pinv[:], op=ALU.mult), [kd, Pinv], [kt_b])
            pb = PB[0]
            for i, src in enumerate((at_b, rt_b, bt_b, kt_b)):
                fw.op("pe", lambda e, i=i, src=src: e.transpose(pb[:, i * 128:(i + 1) * 128], src[:], ident_b[:]), reads=[src, ident_b], writes=[pb])
            dve(lambda e: e.tensor_copy(arbk[:], pb[:, 0:512]), [pb], [arbk])
            Rf = []
            for h in range(2):
                ps = PS[h]
                mm(ps[:, 0:256], arbk[hs[h], 256:384], arbk[hs[h], 0:256], True, True, [arbk], [ps])
                mm(ps[:, 256:512], arbk[hs[h], 384:512], arbk[hs[h], 0:256], True, True, [arbk], [ps])
                dve(lambda e, h=h, ps=ps: e.tensor_tensor(out=Am[h][:, 0:256], in0=ps[:, 0:256], in1=msk[:, d, :], op=ALU.mult), [ps, msk], [Am[h]])
                dve(lambda e, h=h, ps=ps: e.tensor_tensor(out=Am[h][:, 256:512], in0=ps[:, 256:512], in1=msk[:, d, :], op=ALU.mult), [ps, msk], [Am[h]])
                ps = PS[2 + h]
                mm(ps[:, 0:128], arbk[hs[h], 0:128], arbk[hs[h], 256:384], True, True, [arbk], [ps])
                N, NT, R = Nn[h][0], NTt[h][0], Rr[h][0]
                dve(lambda e, NT=NT, ps=ps: e.tensor_tensor(out=NT[:], in0=ps[:, 0:128], in1=mskT[:, d, :], op=ALU.mult), [ps, mskT], [NT])
                pool(lambda e, N=N, h=h: e.tensor_copy(N[:], Am[h][:, 0:128]), [Am[h]], [N])
                pool(lambda e, R=R, h=h: e.tensor_tensor(out=R[:], in0=Am[h][:, 0:128], in1=ident_f[:], op=ALU.add), [Am[h], ident_f], [R])
                for it in range(6):
                    N2, NT2, R2 = Nn[h][(it + 1) % 2], NTt[h][(it + 1) % 2], Rr[h][(it + 1) % 2]
                    psn = PS[2 + h]
                    if it < 5:
                        mm(psn[:, 0:128], NT[:], N[:], True, True, [NT, N], [psn])
                    mm(psn[:, 128:256], N[:], NT[:], True, True, [NT, N], [psn])
                    if it < 5:
                        act(lambda e, N2=N2, psn=psn: e.copy(N2[:], psn[:, 0:128]), [psn], [N2])
                    act(lambda e, NT2=NT2, psn=psn: e.copy(NT2[:], psn[:, 128:256]), [psn], [NT2])
                    mm(psn[:, 256:384], NT2[:], R[:], True, True, [NT2, R], [psn])
                    dve(lambda e, R2=R2, R=R, psn=psn: e.tensor_tensor(out=R2[:], in0=psn[:, 256:384], in1=R[:], op=ALU.add), [psn, R], [R2])
                    N, NT, R = N2, NT2, R2
                Rf.append(R)
            psx = PS[4]
            mm(psx[:, 0:128], arbk[:, 0:128], STb[:], True, False, [arbk, STb], [psx])
            for h in range(2):
                mm(psx[:, hs[h]], Am[h][:, 256:384], v_b[:, hs[h]], False, h == 1, [Am[h], v_b], [psx])
            act(lambda e: e.copy(XTs[:], psx[:, 0:128]), [psx], [XTs])
            for h in range(2):
                mm(psx[:, 128 + 64 * h:192 + 64 * h], Rf[h][:], XTs[:, hs[h]], True, True, [Rf[h], XTs], [psx])
            act(lambda e: e.copy(UTs[:], psx[:, 128:256]), [psx], [UTs])
            psy = PS[5]
            mm(psy[:, 0:128], arbk[:, 128:256], STb[:], True, False, [arbk, STb], [psy])
            for h in range(2):
                mm(psy[:, hs[h]], Am[h][:, 128:256], UTs[:, hs[h]], False, False, [Am[h], UTs], [psy])
                mm(psy[:, hs[h]], Am[h][:, 384:512], v_b[:, hs[h]], False, h == 1, [Am[h], v_b], [psy])
            mm(psx[:, 256:384], bt_b[:], UTs[:], True, False, [bt_b, UTs], [psx])
            mm(psx[:, 256:384], kt_b[:], v_b[:], False, True, [kt_b, v_b], [psx])
            dve(lambda e: e.tensor_tensor(out=S32[:], in0=psx[:, 256:384], in1=S32[:], op=ALU.add), [psx, S32], [S32])
            dve(lambda e: e.scalar_tensor_tensor(out=S32[:], in0=S32[:], scalar=ptot[:, 0:1], in1=blockmask[:], op0=ALU.mult, op1=ALU.mult), [S32, ptot, blockmask], [S32])
            act(lambda e: e.copy(STb[:], S32[:]), [S32], [STb])
            if d == 0:
                dve(lambda e: e.tensor_copy(yb[:, 0:128], psy[:, 0:128]), [psy], [yb])
                ld(YF[g0:g0 + 128, :], yb[:], [yb], [YF])
            else:
                ld(yf[:], YF[g0:g0 + 128, :], [YF], [yf])
                dve(lambda e: e.tensor_tensor(out=yf[:, 0:128], in0=psy[:, 0:128], in1=yf[:, 0:128], op=ALU.add), [psy, yf], [yf])
                dve(lambda e: e.tensor_tensor(out=yf[:, 128:256], in0=yb[:, 128:256], in1=yf[:, 128:256], op=ALU.add), [yb, yf], [yf])
                for h in range(2):
                    dve(lambda e, h=h: e.bn_stats(bnst[:, h, :], yf[:, hs[h]]), [yf], [bnst])
                    dve(lambda e, h=h: e.bn_aggr(bnag[:, h, :], bnst[:, h, :]), [bnst], [bnag])
                dve(lambda e: e.tensor_scalar(out=ss[:, 6:8], in0=bnag[:, :, 1], scalar1=64e-5, scalar2=None, op0=ALU.add), [bnag], [ss])
                act(lambda e: e.activation(out=ss[:, 6:8], in_=ss[:, 6:8], func=AF.Sqrt), [ss], [ss])
                dve(lambda e: e.reciprocal(ss[:, 6:8], ss[:, 6:8]), [ss], [ss])
                for h in range(2):
                    dve(lambda e, h=h: e.tensor_scalar(out=gno[:, hs[h]], in0=yf[:, hs[h]], scalar1=bnag[:, h, 0:1], scalar2=ss[:, 6 + h:7 + h],
                                                       op0=ALU.subtract, op1=ALU.mult), [yf, bnag, ss], [gno])
                dve(lambda e: e.tensor_tensor(out=gno[:], in0=gno[:], in1=prm[:, 7, :], op=ALU.mult), [gno, prm], [gno])
                dve(lambda e: e.tensor_tensor(out=gno[:], in0=gno[:], in1=prm[:, 8, :], op=ALU.add), [gno, prm], [gno])
                dve(lambda e: e.tensor_tensor(out=gno[:], in0=gno[:], in1=yf[:, 128:256], op=ALU.add), [gno, yf], [gno])
                act(lambda e: e.activation(out=siggd[:], in_=fmg[5][:], func=AF.Sigmoid), [fmg[5]], [siggd])
                psg = PS[3]
                mm(psg[:, 0:128], siggd[:], g2b[:], True, True, [siggd, g2b], [psg])
                dve(lambda e: e.tensor_tensor(out=gnob[:], in0=psg[:, 0:128], in1=gno[:], op=ALU.mult), [psg, gno], [gnob])
                pb1 = PB[1]
                fw.op("pe", lambda e: e.transpose(pb1[:, 0:128], gnob[:], ident_b[:]), reads=[gnob, ident_b], writes=[pb1])
                act(lambda e: e.copy(ytb[:], pb1[:, 0:128]), [pb1], [ytb])
                ld(YTl[0:128, g0:g0 + 128], ytb[:], [ytb], [YTl])

        def zero_state():
            pool(lambda e: e.memset(S32[:], 0.0), [], [S32])
            pool(lambda e: e.memset(STb[:], 0.0), [], [STb])

        soff = [0]
        for L in seqs:
            soff.append(soff[-1] + L)
        for si, L in enumerate(seqs):
            s0, s1 = soff[si], soff[si + 1]
            for d in range(2):
                zero_state()
                chunks = list(range(s0, s1, 128))
                if d == 1:
                    chunks = chunks[::-1]
                for g0 in chunks:
                    rwkv_chunk(d, g0, s0, s1)

        krot = [TB("krot%d" % i) for i in range(4)]
        vtm = [TB("vtm%d" % i) for i in range(4)]
        qrot = TB("qrot")
        ropc, rops = T32("ropc"), T32("rops")
        sc = T32("sc", 384)
        pn = TB("pn", 384)
        pT = fw.sbuf("pT", [128, 3, 128], BF16)
        tmpq = T32("tmpq")
        otb = T32("otb")
        am = fw.sbuf("am", [128, 8], F32)
        SCALE = 128.0 ** -0.5

        def att_proj_fm(xt, c0, dst, ropc_, rops_):
            psa, psb = PS[0], PS[1]
            for kc in range(KC):
                mm(psa[:, 0:128], Wall[:, kc, c0:c0 + 128], xt[:, kc, 1:129], kc == 0, kc == KC - 1, [Wall, xt], [psa])
            for kc in range(KC):
                mm(psb[:, 0:128], Wall[:, kc, c0 + 128:c0 + 256], xt[:, kc, 1:129], kc == 0, kc == KC - 1, [Wall, xt], [psb])
            dve(lambda e: e.tensor_tensor(out=tmpq[:], in0=psa[:, 0:128], in1=ropc_[:], op=ALU.mult), [psa, ropc_], [tmpq])
            dve(lambda e: e.tensor_tensor(out=sqk[:], in0=psb[:, 0:128], in1=rops_[:], op=ALU.mult), [psb, rops_], [sqk])
            dve(lambda e: e.tensor_tensor(out=dst[:], in0=tmpq[:], in1=sqk[:], op=ALU.add), [tmpq, sqk], [dst])

        def att_kv(g0, s0, s1, slot):
            xt = load_xt(g0, s0, s1)
            ld(ropc[:], ropec[:, g0 - s0:g0 - s0 + 128], [ropec], [ropc])
            ld(rops[:], ropes[:, g0 - s0:g0 - s0 + 128], [ropes], [rops])
            att_proj_fm(xt, 1024, krot[slot], ropc, rops)
            ps = PS[2]
            for kc in range(KC):
                mm(ps[:, 0:128], xt[:, kc, 1:129], Wall[:, kc, 1280:1408], kc == 0, kc == KC - 1, [Wall, xt], [ps])
            act(lambda e: e.copy(vtm[slot][:], ps[:, 0:128]), [ps], [vtm[slot]])
            return xt

        for si, L in enumerate(seqs):
            s0, s1 = soff[si], soff[si + 1]
            nb = L // 128
            att_kv(s0, s0, s1, 0)
            for b in range(nb):
                g0 = s0 + b * 128
                xt = load_xt(g0, s0, s1)
                ld(ropc[:], ropec[:, b * 128:(b + 1) * 128], [ropec], [ropc])
                ld(rops[:], ropes[:, b * 128:(b + 1) * 128], [ropes], [rops])
                att_proj_fm(xt, 768, qrot, ropc, rops)
                if b + 1 < nb:
                    att_kv(g0 + 128, s0, s1, (b + 1) % 4)
                js = [j for j in (-1, 0, 1) if 0 <= b + j < nb]
                c_lo, c_hi = (js[0] + 1) * 128, (js[-1] + 2) * 128
                pss = PS[3]
                for j in js:
                    mm(pss[:, (j + 1) * 128:(j + 2) * 128], qrot[:], krot[(b + j) % 4][:], True, True, [qrot, krot[(b + j) % 4]], [pss])
                dve(lambda e, c_lo=c_lo, c_hi=c_hi: e.scalar_tensor_tensor(out=sc[:, c_lo:c_hi], in0=pss[:, c_lo:c_hi], scalar=SCALE, in1=attmask[:, c_lo:c_hi],
                                                                           op0=ALU.mult, op1=ALU.add), [pss, attmask], [sc])
                dve(lambda e, c_lo=c_lo, c_hi=c_hi: e.tensor_reduce(out=am[:, 0:1], in_=sc[:, c_lo:c_hi], axis=AX.X, op=ALU.max), [sc], [am])
                dve(lambda e: e.tensor_tensor(out=am[:, 0:1], in0=am[:, 0:1], in1=sinkb, op=ALU.max), [am, prm], [am])
                dve(lambda e: e.tensor_scalar(out=am[:, 1:2], in0=am[:, 0:1], scalar1=-1.0, scalar2=None, op0=ALU.mult), [am], [am])
                act(lambda e, c_lo=c_lo, c_hi=c_hi: e.activation(out=sc[:, c_lo:c_hi], in_=sc[:, c_lo:c_hi], func=AF.Exp, bias=am[:, 1:2], accum_out=am[:, 2:3]), [sc, am], [sc, am])
                act(lambda e: e.activation(out=am[:, 3:4], in_=sinkb, func=AF.Exp, bias=am[:, 1:2]), [prm, am], [am])
                dve(lambda e: e.tensor_tensor(out=am[:, 4:5], in0=am[:, 2:3], in1=am[:, 3:4], op=ALU.add), [am], [am])
                dve(lambda e: e.reciprocal(am[:, 5:6], am[:, 4:5]), [am], [am])
                dve(lambda e, c_lo=c_lo, c_hi=c_hi: e.tensor_scalar(out=pn[:, c_lo:c_hi], in0=sc[:, c_lo:c_hi], scalar1=am[:, 5:6], scalar2=None, op0=ALU.mult), [sc, am], [pn])
                pb1 = PB[1]
                for j in js:
                    fw.op("pe", lambda e, j=j: e.transpose(pb1[:, (j + 1) * 128:(j + 2) * 128], pn[:, (j + 1) * 128:(j + 2) * 128], ident_b[:]), reads=[pn, ident_b], writes=[pb1])
                dve(lambda e, c_lo=c_lo, c_hi=c_hi: e.tensor_copy(pT[:].rearrange("p j q -> p (j q)")[:, c_lo:c_hi], pb1[:, c_lo:c_hi]), [pb1], [pT])
                pso = PS[2]
                for j in js:
                    mm(pso[:, 128:256], vtm[(b + j) % 4][:], pT[:, j + 1, :], j == js[0], j == js[-1], [vtm[(b + j) % 4], pT], [pso])
                act(lambda e: e.copy(otb[:], pso[:, 128:256]), [pso], [otb])
                ld(YTl[128:256, g0:g0 + 128], otb[:], [otb], [YTl])
        fw.emit()
        es.close()
        return nc
    if phase == "B1":
        x1all = din("xall", [D, NT])
        w1_in = din("w1", [D, 640])
        w1b = fw.dram("w1b", [D, 640], BF16)
        castdma(w1b, w1_in, D)
        YTl = dout("yt", [256, NT])
        HF = fw.dram("HF", [128, NT], F32)
        W1 = fw.sbuf("W1", [128, KC, 640], BF16)
        ld(W1[:], w1b.t.ap().rearrange("(kc p) n -> p kc n", p=128), [w1b], [W1])
        p1 = cload("p1", prm1, [128, 16])
        lwr32 = cload("lwr32", lwr_in, [128, 2, 128])
        lwi32 = cload("lwi32", lwi_in, [128, 2, 128])
        lwrb = fw.sbuf("lwrb", [128, 2, 128], BF16)
        lwib = fw.sbuf("lwib", [128, 2, 128], BF16)
        dve(lambda e: e.tensor_copy(lwrb[:], lwr32[:]), [lwr32], [lwrb])
        dve(lambda e: e.tensor_copy(lwib[:], lwi32[:]), [lwi32], [lwib])
        spl = fw.sbuf("spl", [128, 4], F32)
        for d in range(2):
            act(lambda e, d=d: e.activation(out=spl[:, 2 + d:3 + d], in_=p1[:, 12 + d:13 + d], func=AF.Exp, scale=-1.0), [p1], [spl])
            act(lambda e, d=d: e.activation(out=spl[:, 2 + d:3 + d], in_=spl[:, 2 + d:3 + d], func=AF.Ln, bias=1.0), [spl], [spl])
            dve(lambda e, d=d: e.tensor_scalar(out=spl[:, d:d + 1], in0=spl[:, 2 + d:3 + d], scalar1=-8.0, scalar2=None, op0=ALU.mult), [spl], [spl])
            dve(lambda e, d=d: e.tensor_scalar(out=spl[:, 2 + d:3 + d], in0=spl[:, d:d + 1], scalar1=2.0, scalar2=None, op0=ALU.mult), [spl], [spl])
        TL = 512
        XTv1 = x1all.t.ap().rearrange("(kc p) t -> p kc t", p=128)
        xl = [fw.sbuf("xl%d" % i, [128, KC, TL], BF16) for i in range(2)]
        HAL = 4
        pj = [fw.sbuf("pj%d" % g, [128, TL + 2 * HAL], F32) for g in range(5)]
        gch = fw.sbuf("gch", [128, TL + 2 * HAL], F32)
        xc = fw.sbuf("xc", [128, TL], F32)
        xcb = fw.sbuf("xcb", [128, TL], BF16)
        rg, ig, aa, bb, hh, tmpl = (fw.sbuf(n, [128, TL], F32) for n in ("rg", "ig", "aa", "bb", "hh", "tmpl"))
        hfl = fw.sbuf("hfl", [128, TL], F32)
        yc = fw.sbuf("yc", [128, TL], F32)
        ydd = fw.sbuf("ydd", [128, TL], F32)
        hlast = fw.sbuf("hlast", [128, 2], F32)

        def lru_tile(d, g0, s0, s1, first):
            lo, hi = max(s0, g0 - 2), min(s1, g0 + TL + 2)
            for g in range(5):
                if g < 3 and d == 0:
                    continue
                pool(lambda e, g=g: e.memset(pj[g][:], 0.0), [], [pj[g]])
            xt = xl[0]
            segs = []
            if lo < g0:
                segs.append((lo, g0))
            segs.append((g0, g0 + TL))
            if hi > g0 + TL:
                segs.append((g0 + TL, hi))
            for (a, b) in segs:
                n = b - a
                xt = xl[1] if n < TL else xl[0]
                fw.dma(lambda e, xt=xt, a=a, n=n: e.dma_start(out=xt[:, :, 0:n], in_=XTv1[:, :, a:a + n], allow_slow_non_contiguous=True),
                       reads=[x1all], writes=[xt], eng="pool")
                off = HAL + (a - g0)
                for g in range(5):
                    if g < 3 and d == 0:
                        continue
                    ps = PS[g % 4]
                    for kc in range(KC):
                        mm(ps[:, 0:n], W1[:, kc, g * 128:(g + 1) * 128], xt[:, kc, 0:n], kc == 0, kc == KC - 1, [W1, xt], [ps])
                    act(lambda e, g=g, ps=ps, off=off, n=n: e.copy(pj[g][:, off:off + n], ps[:, 0:n]), [ps], [pj[g]])
            C = slice(HAL, HAL + TL)
            dve(lambda e: e.tensor_scalar(out=xc[:], in0=pj[3][:, HAL - 2:HAL - 2 + TL], scalar1=p1[:, 3:4], scalar2=p1[:, 7:8], op0=ALU.mult, op1=ALU.add), [pj[3], p1], [xc])
            for j in range(1, 4):
                dve(lambda e, j=j: e.scalar_tensor_tensor(out=xc[:], in0=pj[3][:, HAL - 2 + j:HAL - 2 + j + TL], scalar=p1[:, 3 + j:4 + j], in1=xc[:], op0=ALU.mult, op1=ALU.add), [pj[3], p1, xc], [xc])
            act(lambda e: e.copy(xcb[:], xc[:]), [xc], [xcb])
            psr, psi = PS[4], PS[5]
            mm(psr[:, 0:TL], lwrb[:, d, :], xcb[:], True, True, [lwrb, xcb], [psr])
            mm(psi[:, 0:TL], lwib[:, d, :], xcb[:], True, True, [lwib, xcb], [psi])
            act(lambda e: e.activation(out=rg[:], in_=psr[:, 0:TL], func=AF.Sigmoid, bias=p1[:, 8 + d:9 + d]), [psr, p1], [rg])
            act(lambda e: e.activation(out=ig[:], in_=psi[:, 0:TL], func=AF.Sigmoid, bias=p1[:, 10 + d:11 + d]), [psi, p1], [ig])
            act(lambda e: e.activation(out=aa[:], in_=rg[:], func=AF.Exp, scale=spl[:, d:d + 1]), [rg, spl], [aa])
            act(lambda e: e.activation(out=tmpl[:], in_=rg[:], func=AF.Exp, scale=spl[:, 2 + d:3 + d]), [rg, spl], [tmpl])
            dve(lambda e: e.tensor_scalar(out=tmpl[:], in0=tmpl[:], scalar1=-1.0, scalar2=1.0, op0=ALU.mult, op1=ALU.add), [tmpl], [tmpl])
            dve(lambda e: e.tensor_scalar(out=tmpl[:], in0=tmpl[:], scalar1=0.0, scalar2=None, op0=ALU.max), [tmpl], [tmpl])
            act(lambda e: e.activation(out=tmpl[:], in_=tmpl[:], func=AF.Sqrt), [tmpl], [tmpl])
            dve(lambda e: e.tensor_tensor(out=bb[:], in0=ig[:], in1=xc[:], op=ALU.mult), [ig, xc], [bb])
            dve(lambda e: e.tensor_tensor(out=bb[:], in0=bb[:], in1=tmpl[:], op=ALU.mult), [bb, tmpl], [bb])
            init = 0.0 if first else hlast[:, d:d + 1]
            if d == 0:
                dve(lambda e: e.tensor_tensor_scan(out=hh[:], data0=aa[:], data1=bb[:], initial=init, op0=ALU.mult, op1=ALU.add), [aa, bb, hlast], [hh])
                dve(lambda e: e.tensor_copy(hlast[:, 0:1], hh[:, TL - 1:TL]), [hh], [hlast])
                ld(HF[:, g0:g0 + TL], hh[:], [hh], [HF])
            else:
                dve(lambda e: e.tensor_tensor_scan(out=hh[:, ::-1], data0=aa[:, ::-1], data1=bb[:, ::-1], initial=init, op0=ALU.mult, op1=ALU.add), [aa, bb, hlast], [hh])
                dve(lambda e: e.tensor_copy(hlast[:, 1:2], hh[:, 0:1]), [hh], [hlast])
                ld(hfl[:], HF[:, g0:g0 + TL], [HF], [hfl])
                dve(lambda e: e.tensor_tensor(out=hh[:], in0=hh[:], in1=hfl[:], op=ALU.add), [hh, hfl], [hh])
                act(lambda e: e.activation(out=tmpl[:], in_=pj[4][:, C], func=AF.Gelu), [pj[4]], [tmpl])
                dve(lambda e: e.tensor_tensor(out=ydd[:], in0=hh[:], in1=tmpl[:], op=ALU.mult), [hh, tmpl], [ydd])
                ld(YTl[128:256, g0:g0 + TL], ydd[:], [ydd], [YTl])
                dve(lambda e: e.tensor_tensor(out=gch[:], in0=pj[2][:], in1=pj[0][:], op=ALU.mult), [pj[2], pj[0]], [gch])
                dve(lambda e: e.tensor_scalar(out=tmpl[:], in0=gch[:, HAL - 1:HAL - 1 + TL], scalar1=p1[:, 0:1], scalar2=None, op0=ALU.mult), [gch, p1], [tmpl])
                for j in range(1, 3):
                    dve(lambda e, j=j: e.scalar_tensor_tensor(out=tmpl[:], in0=gch[:, HAL - 1 + j:HAL - 1 + j + TL], scalar=p1[:, j:j + 1], in1=tmpl[:], op0=ALU.mult, op1=ALU.add), [gch, p1, tmpl], [tmpl])
                dve(lambda e: e.tensor_tensor(out=yc[:], in0=tmpl[:], in1=pj[1][:, C], op=ALU.mult), [tmpl, pj[1]], [yc])
                ld(YTl[0:128, g0:g0 + TL], yc[:], [yc], [YTl])

        for si, L in enumerate(seqs):
            s0, s1 = soff[si], soff[si + 1]
            for d in range(2):
                tiles = list(range(s0, s1, TL))
                if d == 1:
                    tiles = tiles[::-1]
                for i, g0 in enumerate(tiles):
                    lru_tile(d, g0, s0, s1, i == 0)
        fw.emit()
        es.close()
        return nc
    DN_ALPHA = 4.0 ** 0.25
    E8_in = bv("e8", [8, 8, 128], rows=8)
    sel = None
    NF = max(F0 // 128, FE // 128, KC)

    def token_phase(layer, last, part=None):
        NE = 8 if part is None else 4
        ex0 = 4 if part == "b" else 0
        if part != "b":
            xT = din("xT", [D, tpc])
            yT = din("yT", [D, tpc])
            wo_in = din("wo", [D, D])
            wo = fw.dram("wo_b", [D, D], BF16)
            castdma(wo, wo_in, D)
        else:
            xm_in = din("xm", [D, tpc])
            zp_in = din("zp", [D, tpc])
            gt_in = din("gt", [8, tpc])
        if layer == 0:
            fgu_in = din("fgu", [D, 2 * F0])
            fdn_in = din("fdn", [F0, D])
            fgu_a = fw.dram("fgu_b", [D, 2 * F0], BF16)
            fdn_a = fw.dram("fdn_b", [F0, D], BF16)
            castdma(fgu_a, fgu_in, D)
            castdma(fdn_a, fdn_in, F0)
            x1T = dout("x1T", [D, tpc])
        else:
            egu_in = din("egu", [NE * D, 2 * FE])
            edn_in = din("edn", [NE * FE, D])
            egu_e = [fw.dram("egu_b%d" % e_, [D, 2 * FE], BF16) for e_ in range(NE)]
            edn_e = [fw.dram("edn_b%d" % e_, [FE, D], BF16) for e_ in range(NE)]
            for e_ in range(NE):
                src_g = Buf(egu_in.t.ap()[e_ * D:(e_ + 1) * D, :], "egu_src")
                src_d = Buf(edn_in.t.ap()[e_ * FE:(e_ + 1) * FE, :], "edn_src")
                castdma(egu_e[e_], src_g, D)
                castdma(edn_e[e_], src_d, FE)
            if part == "a":
                xm_o = dout("xm", [D, tpc])
                zp_o = dout("zp", [D, tpc])
                gt_o = dout("gt", [8, tpc])
            else:
                y_out = dout("y", [tpc, D])
        zt = fw.sbuf("zt", [128, KC, TT], F32)
        xmb = fw.sbuf("xmb", [128, KC, TT], BF16)
        sq = fw.sbuf("sq", [128, TT], F32)
        stat = fw.sbuf("stat", [128, 4, TT], F32)
        ytile = fw.sbuf("ytile", [128, KC, TT], BF16)
        hbuf = fw.sbuf("hbuf", [128, NF, TT], BF16)
        sg = fw.sbuf("sg", [128, TT], F32)
        wbuf = [fw.sbuf("wbuf%d" % i, [128, NF, 128], BF16) for i in range(3)]
        wctr = [0]

        def lnp_col(gb, which, kc):
            i = ((gb * 2 + layer) * 2 + which) * KC + kc
            return lnp_s[:, i:i + 1]

        def load_w(Wd, nk, c0, row0=0):
            b = wbuf[wctr[0] % 3]
            wctr[0] += 1
            src = Wd.t.ap()[row0:row0 + nk * 128, :].rearrange("(kc p) n -> p kc n", p=128)[:, :, c0:c0 + 128]
            ld(b[:, 0:nk, :], src, [Wd], [b])
            return b

        def layer_norm_fm(which):
            psm, psq = PS[4], PS[5]
            for kc in range(KC):
                mm(psm[:, 0:TT], ones_f[:], zt[:, kc, :], kc == 0, kc == KC - 1, [ones_f, zt], [psm])
            for kc in range(KC):
                pool(lambda e, kc=kc: e.tensor_tensor(out=sq[:], in0=zt[:, kc, :], in1=zt[:, kc, :], op=ALU.mult), [zt], [sq])
                mm(psq[:, 0:TT], ones_f[:], sq[:], kc == 0, kc == KC - 1, [ones_f, sq], [psq])
            dve(lambda e: e.tensor_scalar(out=stat[:, 0, :], in0=psm[:, 0:TT], scalar1=1.0 / D, scalar2=None, op0=ALU.mult), [psm], [stat])
            dve(lambda e: e.tensor_scalar(out=stat[:, 1, :], in0=psq[:, 0:TT], scalar1=1.0 / D, scalar2=None, op0=ALU.mult), [psq], [stat])
            dve(lambda e: e.tensor_tensor(out=stat[:, 2, :], in0=stat[:, 0, :], in1=stat[:, 0, :], op=ALU.mult), [stat], [stat])
            dve(lambda e: e.tensor_tensor(out=stat[:, 1, :], in0=stat[:, 1, :], in1=stat[:, 2, :], op=ALU.subtract), [stat], [stat])
            dve(lambda e: e.tensor_scalar(out=stat[:, 1, :], in0=stat[:, 1, :], scalar1=1e-5, scalar2=None, op0=ALU.add), [stat], [stat])
            act(lambda e: e.activation(out=stat[:, 2, :], in_=stat[:, 1, :], func=AF.Sqrt), [stat], [stat])
            dve(lambda e: e.reciprocal(stat[:, 1, :], stat[:, 2, :]), [stat], [stat])
            for kc in range(KC):
                dve(lambda e, kc=kc: e.tensor_tensor(out=zt[:, kc, :], in0=zt[:, kc, :], in1=stat[:, 0, :], op=ALU.subtract), [zt, stat], [zt])
                dve(lambda e, kc=kc: e.tensor_tensor(out=zt[:, kc, :], in0=zt[:, kc, :], in1=stat[:, 1, :], op=ALU.mult), [zt, stat], [zt])
                dve(lambda e, kc=kc: e.tensor_scalar(out=zt[:, kc, :], in0=zt[:, kc, :], scalar1=lnp_col(0, which, kc),
                                                     scalar2=lnp_col(1, which, kc), op0=ALU.mult, op1=ALU.add), [zt, lnp_s], [zt])
            act(lambda e: e.copy(xmb[:], zt[:]), [zt], [xmb])

        def swiglu_h(Wgu, Fd, row0):
            for f in range(Fd // 128):
                wg = load_w(Wgu, KC, f * 128, row0)
                wu = load_w(Wgu, KC, Fd + f * 128, row0)
                pg, pu = PS[f % 2], PS[2 + f % 2]
                for kc in range(KC):
                    mm(pg[:, 0:TT], wg[:, kc, :], xmb[:, kc, :], kc == 0, kc == KC - 1, [wg, xmb], [pg])
                for kc in range(KC):
                    mm(pu[:, 0:TT], wu[:, kc, :], xmb[:, kc, :], kc == 0, kc == KC - 1, [wu, xmb], [pu])
                act(lambda e, pg=pg: e.activation(out=sg[:], in_=pg[:, 0:TT], func=AF.Silu), [pg], [sg])
                dve(lambda e, f=f, pu=pu: e.tensor_tensor(out=hbuf[:, f, :], in0=sg[:], in1=pu[:, 0:TT], op=ALU.mult), [sg, pu], [hbuf])

        fmv = lambda b_: b_.t.ap().rearrange("(kc p) t -> p kc t", p=128)
        if part != "b":
            yTv = fmv(yT)
            x32v = fmv(xT)
        if layer == 1:
            rt32 = fw.sbuf("rt32", [128, KC, 8], F32)
            ld(rt32[:], rt_in[:], [rt_in], [rt32])
            e8 = fw.sbuf("e8", [8, 8, 128], F32)
            ld(e8[:], E8_in[:], [E8_in], [e8])
            lg = fw.sbuf("lg", [128, 8], F32)
            m8 = fw.sbuf("m8", [128, 8], F32)
            gsc = fw.sbuf("gsc", [128, 8], F32)
            gfull = fw.sbuf("gfull", [128, 8], F32)
            gtmp = fw.sbuf("gtmp", [128, 8], F32)
            gT = fw.sbuf("gT", [8, TT], F32)
            gbc = fw.sbuf("gbc", [128, TT], F32)
            otm = fw.sbuf("otm", [128, D], F32)
        for t0 in range(0, tpc, TT):
            if part != "b":
                ldc(ytile[:], yTv[:, :, t0:t0 + TT], [yT], [ytile])
                ld(zt[:], x32v[:, :, t0:t0 + TT], [xT], [zt])
                for oc in range(KC):
                    wb = load_w(wo, KC, oc * 128)
                    ps = PS[oc % 4]
                    for kc in range(KC):
                        mm(ps[:, 0:TT], wb[:, kc, :], ytile[:, kc, :], kc == 0, kc == KC - 1, [wb, ytile], [ps])
                    dve(lambda e, oc=oc, ps=ps: e.scalar_tensor_tensor(out=zt[:, oc, :], in0=zt[:, oc, :], scalar=DN_ALPHA, in1=ps[:, 0:TT],
                                                                       op0=ALU.mult, op1=ALU.add), [zt, ps], [zt])
                layer_norm_fm(0)
            else:
                ld(zt[:], fmv(xm_in)[:, :, t0:t0 + TT], [xm_in], [zt])
                act(lambda e: e.copy(xmb[:], zt[:]), [zt], [xmb])
                ld(zt[:], fmv(zp_in)[:, :, t0:t0 + TT], [zp_in], [zt])
                ld(gT[:], gt_in[:, t0:t0 + TT], [gt_in], [gT])
            if layer == 0:
                swiglu_h(fgu_a, F0, 0)
                for oc in range(KC):
                    wd_ = load_w(fdn_a, F0 // 128, oc * 128)
                    ps = PS[4 + oc % 2]
                    for f in range(F0 // 128):
                        mm(ps[:, 0:TT], wd_[:, f, :], hbuf[:, f, :], f == 0, f == F0 // 128 - 1, [wd_, hbuf], [ps])
                    dve(lambda e, oc=oc, ps=ps: e.scalar_tensor_tensor(out=zt[:, oc, :], in0=zt[:, oc, :], scalar=DN_ALPHA, in1=ps[:, 0:TT],
                                                                       op0=ALU.mult, op1=ALU.add), [zt, ps], [zt])
            else:
                if part != "b":
                    for sb in range(TT // 128):
                        psr = PS[4]
                        for kc in range(KC):
                            mm(psr[:, 0:8], zt[:, kc, sb * 128:(sb + 1) * 128], rt32[:, kc, :], kc == 0, kc == KC - 1, [zt, rt32], [psr])
                        dve(lambda e: e.tensor_copy(lg[:], psr[:, 0:8]), [psr], [lg])
                        dve(lambda e: e.max(m8[:], lg[:]), [lg], [m8])
                        dve(lambda e: e.tensor_tensor(out=gsc[:, 0:1], in0=m8[:, 1:2], in1=m8[:, 0:1], op=ALU.subtract), [m8], [gsc])
                        act(lambda e: e.activation(out=gsc[:, 1:2], in_=gsc[:, 0:1], func=AF.Exp), [gsc], [gsc])
                        dve(lambda e: e.tensor_scalar(out=gsc[:, 1:2], in0=gsc[:, 1:2], scalar1=1.0, scalar2=None, op0=ALU.add), [gsc], [gsc])
                        dve(lambda e: e.reciprocal(gsc[:, 2:3], gsc[:, 1:2]), [gsc], [gsc])
                        dve(lambda e: e.tensor_scalar(out=gsc[:, 3:4], in0=gsc[:, 2:3], scalar1=-1.0, scalar2=1.0, op0=ALU.mult, op1=ALU.add), [gsc], [gsc])
                        dve(lambda e: e.tensor_scalar(out=gfull[:], in0=lg[:], scalar1=m8[:, 0:1], scalar2=gsc[:, 2:3], op0=ALU.is_equal, op1=ALU.mult), [lg, m8, gsc], [gfull])
                        dve(lambda e: e.tensor_scalar(out=gtmp[:], in0=lg[:], scalar1=m8[:, 1:2], scalar2=gsc[:, 3:4], op0=ALU.is_equal, op1=ALU.mult), [lg, m8, gsc], [gtmp])
                        dve(lambda e: e.tensor_tensor(out=gfull[:], in0=gfull[:], in1=gtmp[:], op=ALU.add), [gfull, gtmp], [gfull])
                        fw.op("pe", lambda e: e.transpose(psr[0:8, 128:256], gfull[:], ident_f[:]), reads=[gfull, ident_f], writes=[psr])
                        dve(lambda e, sb=sb: e.tensor_copy(gT[:, sb * 128:(sb + 1) * 128], psr[0:8, 128:256]), [psr], [gT])
                    if part == "a":
                        ld(fmv(xm_o)[:, :, t0:t0 + TT], zt[:], [zt], [xm_o])
                        ld(gt_o[:, t0:t0 + TT], gT[:], [gT], [gt_o])
                    dve(lambda e: e.tensor_scalar(out=zt[:], in0=zt[:], scalar1=DN_ALPHA, scalar2=None, op0=ALU.mult), [zt], [zt])
                for ex in range(ex0, ex0 + NE):
                    psb_ = PS[4]
                    mm(psb_[:, 0:TT], e8[:, ex, :], gT[:], True, True, [e8, gT], [psb_])
                    dve(lambda e: e.tensor_copy(gbc[:], psb_[:, 0:TT]), [psb_], [gbc])
                    swiglu_h(egu_e[ex - ex0], FE, 0)
                    for oc in range(KC):
                        wd_ = load_w(edn_e[ex - ex0], FE // 128, oc * 128, 0)
                        ps = PS[4 + oc % 2]
                        for f in range(FE // 128):
                            mm(ps[:, 0:TT], wd_[:, f, :], hbuf[:, f, :], f == 0, f == FE // 128 - 1, [wd_, hbuf], [ps])
                        dve(lambda e, ps=ps: e.tensor_tensor(out=sg[:], in0=ps[:, 0:TT], in1=gbc[:], op=ALU.mult), [ps, gbc], [sg])
                        pool(lambda e, oc=oc: e.tensor_tensor(out=zt[:, oc, :], in0=zt[:, oc, :], in1=sg[:], op=ALU.add), [zt, sg], [zt])
            if part == "a":
                ld(fmv(zp_o)[:, :, t0:t0 + TT], zt[:], [zt], [zp_o])
                continue
            layer_norm_fm(1)
            if not last:
                ld(x1T.t.ap().rearrange("(kc p) t -> p kc t", p=128)[:, :, t0:t0 + TT], zt[:], [zt], [x1T])
            else:
                for sb in range(TT // 128):
                    for g in range(KC // 4):
                        ps = PS[g % 2]
                        for j in range(4):
                            kc = g * 4 + j
                            fw.op("pe", lambda e, ps=ps, j=j, kc=kc, sb=sb: e.transpose(ps[:, j * 128:(j + 1) * 128], zt[:, kc, sb * 128:(sb + 1) * 128], ident_f[:]),
                                  reads=[zt, ident_f], writes=[ps])
                        act(lambda e, ps=ps, g=g: e.copy(otm[:, g * 512:(g + 1) * 512], ps[:]), [ps], [otm])
                    ld(y_out[t0 + sb * 128:t0 + (sb + 1) * 128, :], otm[:], [otm], [y_out])
        fw.emit()
        es.close()
        return nc

    if phase == "C0":
        return token_phase(0, False)
    if phase == "C1":
        return token_phase(1, True)
    if phase == "C1a":
        return token_phase(1, True, "a")
    if phase == "C1b":
        return token_phase(1, True, "b")


_CACHE = {}


def _launch(phase, seqs, tpc, F0, FE, TT, in_maps):
    key = (phase, tuple(seqs), F0, FE, TT)
    if key not in _CACHE:
        _CACHE[key] = build_program(seqs, tpc, F0, FE, TT, phase)
    res = run_bass_kernel_spmd(_CACHE[key], in_maps, core_ids=list(range(NCORES)))
    return res.results


def _concat_heads(yts):
    Y = np.empty((D, yts[0].shape[1]), np.float32)
    for r in range(NCORES):
        Y[128 * r:128 * (r + 1)] = yts[r][0:128]
        Y[1024 + 128 * r:1024 + 128 * (r + 1)] = yts[r][128:256]
    return Y


def run_model(seq_arrays, P, F0, FE, TT, upto="C1"):
    seqs = [a.shape[0] for a in seq_arrays]
    X = np.concatenate(seq_arrays, 0).astype(np.float32)
    NT = X.shape[0]
    tpc = NT // NCORES
    lmax = max(seqs)
    cs = _consts(lmax)
    lay, blobM = _blob_layout(lmax)
    f32 = lambda a: np.ascontiguousarray(np.asarray(a, np.float32))
    e8 = np.zeros((8, 8, 128), np.float32)
    for e_ in range(8):
        e8[e_, e_, :] = 1.0
    lnp = np.zeros((128, 8 * KC), np.float32)
    for gb, arr in enumerate((P["ln_g"], P["ln_b"])):
        for layer in range(2):
            for which in range(2):
                i = ((gb * 2 + layer) * 2 + which) * KC
                lnp[:, i:i + KC] = _fm_vec(arr[layer, which])
    w_in, mu = f32(P["ev_w_in"][0]), f32(P["rw_mu"][0])
    od_in = f32(P["od_w_in"][0])
    blobs, w0s, w1s = [], [], []
    for c in range(NCORES):
        ch = slice(128 * c, 128 * c + 128)
        kvh = c // 4
        qc = 3456 + 128 * c
        kc_ = 3456 + 1024 + 128 * kvh
        vc_ = 3456 + 1024 + 256 + 128 * kvh
        swap = np.r_[64:128, 0:64]
        rw_cols = np.r_[128 * c:128 * c + 128, 1024 + 128 * c:1024 + 128 * c + 128, 2048 + 128 * c:2048 + 128 * c + 128, 3072:3456]
        cols = np.r_[rw_cols, qc:qc + 128, qc + swap, kc_:kc_ + 128, kc_ + swap, vc_:vc_ + 128]
        w0s.append(np.ascontiguousarray(w_in[:, cols]))
        mu_fm = np.zeros((128, 12), np.float32)
        for g in range(6):
            mu_fm[:, 2 * g] = mu[0, rw_cols[g * 128:(g + 1) * 128]]
            mu_fm[:, 2 * g + 1] = mu[1, rw_cols[g * 128:(g + 1) * 128]]
        prm0 = np.zeros((16, 128), np.float32)
        prm0[0], prm0[1] = P["rw_w0"][0, 0, ch], P["rw_w0"][0, 1, ch]
        prm0[2], prm0[3] = P["rw_a0"][0, 0, ch], P["rw_a0"][0, 1, ch]
        prm0[4], prm0[5] = P["rw_kk"][0, ch], P["rw_ka"][0, ch]
        prm0[6] = np.asarray(P["rw_rk"][0]).reshape(-1)[ch]
        prm0[7], prm0[8] = P["rw_gn_g"][0, ch], P["rw_gn_b"][0, ch]
        prm0[10, :] = P["att_sink"][0, c]
        w2 = np.concatenate([P["rw_w2"][0, 0][:, ch], P["rw_w2"][0, 1][:, ch]], 0)
        a2 = np.concatenate([P["rw_a2"][0, 0][:, ch], P["rw_a2"][0, 1][:, ch]], 0)
        g2 = P["rw_g2"][0][:, ch]
        w1s.append(np.ascontiguousarray(np.concatenate([od_in[:, g * 1024 + 128 * c:g * 1024 + 128 * c + 128] for g in range(5)], 1)))
        prm1 = np.zeros((128, 16), np.float32)
        prm1[:, 0:3] = np.asarray(P["sc_conv"][0])[:, ch].T
        prm1[:, 3:7] = np.asarray(P["lru_conv"][0])[:, ch].T
        prm1[:, 7] = P["lru_conv_b"][0, ch]
        prm1[:, 8:10] = np.asarray(P["lru_br"][0])[:, ch].T
        prm1[:, 10:12] = np.asarray(P["lru_bi"][0])[:, ch].T
        prm1[:, 12:14] = np.asarray(P["lru_lam"][0])[:, ch].T
        lwr = np.zeros((128, 2, 128), np.float32)
        lwi = np.zeros((128, 2, 128), np.float32)
        for d in range(2):
            for bl in range(2):
                s_ = slice(64 * bl, 64 * bl + 64)
                lwr[s_, d, s_] = P["lru_wr"][0, d, 2 * c + bl]
                lwi[s_, d, s_] = P["lru_wi"][0, d, 2 * c + bl]
        sel = np.zeros((128, NCORES), np.float32)
        sel[:, c] = 1.0
        blob = np.zeros((128, blobM), np.float32)

        def put(name, arr, rows=128):
            c0, n = lay[name]
            blob[0:rows, c0:c0 + n] = np.asarray(arr, np.float32).reshape(rows, n)
        for k_ in ("ident_f", "cums", "msk", "mskT", "blockmask", "attmask", "ones_f", "ropec", "ropes"):
            put(k_, cs[k_])
        put("lnp", lnp)
        put("sel", sel)
        put("mu_fm", mu_fm)
        put("w2", w2)
        put("a2", a2)
        put("g2", g2)
        put("prm1", prm1)
        put("lwr", lwr)
        put("lwi", lwi)
        put("router", np.asarray(P["moe_router"][0], np.float32).reshape(KC, 128, 8).transpose(1, 0, 2))
        put("e8", e8, rows=8)
        put("prm0", prm0, rows=1)
        blobs.append(blob)
    XT = np.ascontiguousarray(X.T)
    sl = lambda A, c: np.ascontiguousarray(A[:, c * tpc:(c + 1) * tpc])
    r = _launch("B0", seqs, tpc, F0, FE, TT, [{"blob": blobs[c], "xall": XT, "w0": w0s[c]} for c in range(NCORES)])
    Y0 = _concat_heads([r[c]["yt"] for c in range(NCORES)])
    if upto == "B0":
        return Y0
    wo0, fgu, fdn = f32(P["ev_w_out"][0]), f32(P["ffn_w_gu"][0]), f32(P["ffn_w_down"][0])
    r = _launch("C0", seqs, tpc, F0, FE, TT, [{"blob": blobs[c], "xT": sl(XT, c), "yT": sl(Y0, c), "wo": wo0, "fgu": fgu, "fdn": fdn}
                                               for c in range(NCORES)])
    X1T = np.concatenate([r[c]["x1T"] for c in range(NCORES)], 1)
    if upto == "C0":
        return X1T
    r = _launch("B1", seqs, tpc, F0, FE, TT, [{"blob": blobs[c], "xall": X1T, "w1": w1s[c]} for c in range(NCORES)])
    Y1 = _concat_heads([r[c]["yt"] for c in range(NCORES)])
    if upto == "B1":
        return Y1
    wo1 = f32(P["od_w_out"][0])
    egu = f32(np.asarray(P["moe_w_gu"][0]).reshape(8 * D, 2 * FE))
    edn = f32(np.asarray(P["moe_w_down"][0]).reshape(8 * FE, D))
    ra = _launch("C1a", seqs, tpc, F0, FE, TT, [{"blob": blobs[c], "xT": sl(X1T, c), "yT": sl(Y1, c), "wo": wo1,
                                                 "egu": egu[:4 * D], "edn": edn[:4 * FE]} for c in range(NCORES)])
    r = _launch("C1b", seqs, tpc, F0, FE, TT, [{"blob": blobs[c], "xm": ra[c]["xm"], "zp": ra[c]["zp"], "gt": ra[c]["gt"],
                                                "egu": egu[4 * D:], "edn": edn[4 * FE:]} for c in range(NCORES)])
    Y = np.concatenate([r[c]["y"] for c in range(NCORES)], 0)
    outs, o = [], 0
    for L in seqs:
        outs.append(Y[o:o + L])
        o += L
    return outs


def kernel(**inputs):
    P = {k: np.asarray(v) for k, v in inputs.items() if k not in ("x_prompt", "x_sample")}
    xp, xs = np.asarray(inputs["x_prompt"]), np.asarray(inputs["x_sample"])
    seq_arrays = [xp[b] for b in range(xp.shape[0])] + [xs[b] for b in range(xs.shape[0])]
    F0 = P["ffn_w_down"].shape[1]
    FE = P["moe_w_down"].shape[2]
    outs = run_model(seq_arrays, P, F0, FE, 512)
    nb = xp.shape[0]
    y_prompt = np.stack(outs[:nb], 0).astype(np.float32)
    y_sample = np.stack(outs[nb:], 0).astype(np.float32)
    return (y_prompt, y_sample)
```
